# Optimizing a Trainium2 kernel written in Bass

```python
import math
import jax, jax.numpy as jnp
from jax import lax
import numpy as np

D_MODEL = 2048
BATCH = 16
SEQ = 2048
DEPTH = 2

GRID_W = 64
CTX_LEN = 256
N_MIXERS = 2
MIXER_ATTN = 0
MIXER_GMLP = 1
N_HEADS = 32
N_KV_HEADS = 4
Q_PER_KV = N_HEADS // N_KV_HEADS
HEAD_DIM = 64
Q_DIM = N_HEADS * HEAD_DIM
KV_DIM = N_KV_HEADS * HEAD_DIM
QKV_DIM = Q_DIM + 2 * KV_DIM
WINDOW = 128
Q_BLOCK = 128
ROPE_BASE = 10000.0
ROPE_AXIS_DIM = HEAD_DIM // 2
NEG_INF = -1e30
CHUNK = 128
GMLP_WIDTH = 2 * D_MODEL
N_GMLP_GROUPS = 8
GMLP_GROUP_DIM = GMLP_WIDTH // N_GMLP_GROUPS
N_EXPERTS = 64
TOP_K = 8
N_EXPERT_GROUPS = 8
EXPERTS_PER_GROUP = N_EXPERTS // N_EXPERT_GROUPS
TOPK_GROUPS = 4
D_EXPERT = D_MODEL // 4
D_SHARED = D_MODEL // 4
ROUTED_SCALE = 2.5
DN_ALPHA = (2 * DEPTH) ** 0.25
DN_BETA = (8 * DEPTH) ** -0.25
LN_EPS = 1e-5
N_ATTN_LAYERS = (DEPTH + 1) // 2
N_GMLP_LAYERS = DEPTH // 2

kernel_name = "hybrid_swa_gmlp_moe_deepnorm_dit"


def _layernorm(x, g, b):
    xf = x.astype(jnp.float32)
    mu = jnp.mean(xf, axis=-1, keepdims=True)
    var = jnp.mean(jnp.square(xf - mu), axis=-1, keepdims=True)
    return ((xf - mu) * lax.rsqrt(var + LN_EPS)).astype(x.dtype) * g + b


def _axial_rope_tables(rows, dtype):
    row_ids = jnp.repeat(jnp.arange(rows, dtype=jnp.float32), GRID_W)
    col_ids = jnp.tile(jnp.arange(GRID_W, dtype=jnp.float32), rows)
    inv_freq = ROPE_BASE ** (-jnp.arange(0, ROPE_AXIS_DIM, 2, dtype=jnp.float32) / ROPE_AXIS_DIM)
    ang = jnp.stack([row_ids, col_ids], axis=-1)[..., None] * inv_freq
    return jnp.cos(ang).astype(dtype), jnp.sin(ang).astype(dtype)


def _rotate(x, cos, sin):
    x1, x2 = jnp.split(x, 2, axis=-1)
    return jnp.concatenate([x1 * cos - x2 * sin, x2 * cos + x1 * sin], axis=-1)


def _axial_rope(x, cos, sin):
    shape = (x.shape[1],) + (1,) * (x.ndim - 3) + (ROPE_AXIS_DIM // 2,)
    x_row, x_col = jnp.split(x, 2, axis=-1)
    return jnp.concatenate([
        _rotate(x_row, cos[:, 0].reshape(shape), sin[:, 0].reshape(shape)),
        _rotate(x_col, cos[:, 1].reshape(shape), sin[:, 1].reshape(shape))], axis=-1)


def _sink_softmax(sink_kg, parts):
    b, h, g, q, _ = parts[0].shape
    s = jnp.broadcast_to(sink_kg[None, :, :, None, None], (b, h, g, q, 1))
    p = jax.nn.softmax(jnp.concatenate([s] + parts, axis=-1), axis=-1)
    splits = [int(v) for v in np.cumsum([1] + [pt.shape[-1] for pt in parts])[:-1]]
    return jnp.split(p, splits, axis=-1)[1:]


def _windowed_gqa(h_lat, h_ctx, w_qkv, w_o, sink, rope_cos, rope_sin, ctx_queries):
    B, S, _ = h_lat.shape
    C = h_ctx.shape[1]
    scale = HEAD_DIM ** -0.5
    sink_kg = sink.reshape(N_KV_HEADS, Q_PER_KV).astype(jnp.float32)
    qkv = h_lat @ w_qkv
    q = qkv[..., :Q_DIM].reshape(B, S, N_KV_HEADS, Q_PER_KV, HEAD_DIM)
    k = qkv[..., Q_DIM:Q_DIM + KV_DIM].reshape(B, S, N_KV_HEADS, HEAD_DIM)
    v = qkv[..., Q_DIM + KV_DIM:].reshape(B, S, N_KV_HEADS, HEAD_DIM)
    q = _axial_rope(q, rope_cos, rope_sin)
    k = _axial_rope(k, rope_cos, rope_sin)
    if ctx_queries:
        qkv_c = h_ctx @ w_qkv
        q_c = qkv_c[..., :Q_DIM].reshape(B, C, N_KV_HEADS, Q_PER_KV, HEAD_DIM)
        kv_c = qkv_c[..., Q_DIM:]
    else:
        kv_c = h_ctx @ w_qkv[:, Q_DIM:]
    k_c = kv_c[..., :KV_DIM].reshape(B, C, N_KV_HEADS, HEAD_DIM)
    v_c = kv_c[..., KV_DIM:].reshape(B, C, N_KV_HEADS, HEAD_DIM)

    n_blocks = S // Q_BLOCK
    win = Q_BLOCK + 2 * WINDOW
    q_blocks = q.reshape(B, n_blocks, Q_BLOCK, N_KV_HEADS, Q_PER_KV, HEAD_DIM).transpose(1, 0, 2, 3, 4, 5)
    pad = ((0, 0), (WINDOW, WINDOW), (0, 0), (0, 0))
    k_pad = jnp.pad(k, pad)
    v_pad = jnp.pad(v, pad)

    def block(args):
        i, qb = args
        start = i * Q_BLOCK
        kw = lax.dynamic_slice_in_dim(k_pad, start, win, axis=1)
        vw = lax.dynamic_slice_in_dim(v_pad, start, win, axis=1)
        q_pos = start + jnp.arange(Q_BLOCK)
        k_pos = start - WINDOW + jnp.arange(win)
        valid = (jnp.abs(q_pos[:, None] - k_pos[None, :]) <= WINDOW) & (k_pos >= 0)[None, :] & (k_pos < S)[None, :]
        s_win = jnp.einsum('bqhgd,bkhd->bhgqk', qb, kw).astype(jnp.float32) * scale
        s_win = jnp.where(valid, s_win, NEG_INF)
        s_ctx = jnp.einsum('bqhgd,bkhd->bhgqk', qb, k_c).astype(jnp.float32) * scale
        p_ctx, p_win = _sink_softmax(sink_kg, [s_ctx, s_win])
        return (jnp.einsum('bhgqk,bkhd->bqhgd', p_ctx.astype(v_c.dtype), v_c)
                + jnp.einsum('bhgqk,bkhd->bqhgd', p_win.astype(vw.dtype), vw))

    o = lax.map(block, (jnp.arange(n_blocks), q_blocks))
    y = o.transpose(1, 0, 2, 3, 4, 5).reshape(B, S, Q_DIM) @ w_o

    y_c = None
    if ctx_queries:
        s_cc = jnp.einsum('bqhgd,bkhd->bhgqk', q_c, k_c).astype(jnp.float32) * scale
        (p_cc,) = _sink_softmax(sink_kg, [s_cc])
        o_c = jnp.einsum('bhgqk,bkhd->bqhgd', p_cc.astype(v_c.dtype), v_c)
        y_c = o_c.reshape(B, C, Q_DIM) @ w_o
    return y, y_c


def _chunk_gmlp(h, w_in, b_in, v_g, v_b, w_s, b_s, w_o):
    B, L, _ = h.shape
    z = jax.nn.gelu(h @ w_in + b_in, approximate=False)
    u, v = jnp.split(z, 2, axis=-1)
    v = _layernorm(v, v_g, v_b)
    vc = v.reshape(B, L // CHUNK, CHUNK, N_GMLP_GROUPS, GMLP_GROUP_DIM)
    mixed = jnp.einsum('gpq,bcqgd->bcpgd', w_s, vc) + b_s.T[:, :, None]
    return (u * mixed.reshape(B, L, GMLP_WIDTH)) @ w_o


def _moe(h, w_router, router_bias, w1, w3, w2, ws1, ws3, ws2):
    T = h.shape[0]
    scores = jax.nn.sigmoid((h @ w_router).astype(jnp.float32))
    biased = scores + router_bias.astype(jnp.float32)
    grp = biased.reshape(T, N_EXPERT_GROUPS, EXPERTS_PER_GROUP)
    grp_score = lax.top_k(grp, 2)[0].sum(-1)
    _, top_grp = lax.top_k(grp_score, TOPK_GROUPS)
    grp_mask = jax.nn.one_hot(top_grp, N_EXPERT_GROUPS, dtype=jnp.float32).sum(1)
    expert_mask = jnp.repeat(grp_mask, EXPERTS_PER_GROUP, axis=1)
    _, top_idx = lax.top_k(jnp.where(expert_mask > 0, biased, -jnp.inf), TOP_K)
    w = jnp.take_along_axis(scores, top_idx, axis=1)
    w = w / jnp.sum(w, axis=-1, keepdims=True) * ROUTED_SCALE
    gates = jnp.sum(jax.nn.one_hot(top_idx, N_EXPERTS, dtype=jnp.float32) * w[..., None], axis=1).astype(h.dtype)

    def expert(e, acc):
        a = jax.nn.silu(h @ w1[e]) * (h @ w3[e])
        return acc + lax.dynamic_slice_in_dim(gates, e, 1, axis=1) * (a @ w2[e])

    routed = lax.fori_loop(0, N_EXPERTS, expert, jnp.zeros_like(h))
    shared = (jax.nn.silu(h @ ws1) * (h @ ws3)) @ ws2
    return routed + shared


def setup_inputs(seed: int = 0) -> dict:
    key = jax.random.key(seed)
    ks = iter(jax.random.split(key, 32))
    f32 = jnp.float32

    def nrm(shape, scale):
        return jax.random.normal(next(ks), shape, f32) * scale

    D = D_MODEL
    return {
        "x": nrm((BATCH, SEQ, D), 1.0),
        "c": nrm((BATCH, D), 1.0),
        "ctx": nrm((BATCH, CTX_LEN, D), 1.0),
        "c_ctx": nrm((D,), 1.0),
        "w_ada": nrm((DEPTH, D, 6 * D), 0.5 * D ** -0.5),
        "b_ada": nrm((DEPTH, 6 * D), 0.02),
        "ln_mix_g": 1.0 + nrm((DEPTH, D), 0.02),
        "ln_mix_b": nrm((DEPTH, D), 0.02),
        "ln_ffn_g": 1.0 + nrm((DEPTH, D), 0.02),
        "ln_ffn_b": nrm((DEPTH, D), 0.02),
        "attn_w_qkv": nrm((N_ATTN_LAYERS, D, QKV_DIM), D ** -0.5),
        "attn_w_o": nrm((N_ATTN_LAYERS, Q_DIM, D), Q_DIM ** -0.5 * DN_BETA),
        "attn_sink": nrm((N_ATTN_LAYERS, N_HEADS), 0.5),
        "gmlp_w_in": nrm((N_GMLP_LAYERS, D, 2 * GMLP_WIDTH), D ** -0.5),
        "gmlp_b_in": nrm((N_GMLP_LAYERS, 2 * GMLP_WIDTH), 0.02),
        "gmlp_v_g": 1.0 + nrm((N_GMLP_LAYERS, GMLP_WIDTH), 0.02),
        "gmlp_v_b": nrm((N_GMLP_LAYERS, GMLP_WIDTH), 0.02),
        "gmlp_w_s": nrm((N_GMLP_LAYERS, N_GMLP_GROUPS, CHUNK, CHUNK), CHUNK ** -0.5),
        "gmlp_b_s": 1.0 + nrm((N_GMLP_LAYERS, N_GMLP_GROUPS, CHUNK), 0.02),
        "gmlp_w_o": nrm((N_GMLP_LAYERS, GMLP_WIDTH, D), GMLP_WIDTH ** -0.5 * DN_BETA),
        "moe_w_router": nrm((DEPTH, D, N_EXPERTS), D ** -0.5),
        "moe_bias": nrm((DEPTH, N_EXPERTS), 0.01),
        "moe_w1": nrm((DEPTH, N_EXPERTS, D, D_EXPERT), D ** -0.5),
        "moe_w3": nrm((DEPTH, N_EXPERTS, D, D_EXPERT), D ** -0.5),
        "moe_w2": nrm((DEPTH, N_EXPERTS, D_EXPERT, D), D_EXPERT ** -0.5 * DN_BETA),
        "moe_ws1": nrm((DEPTH, D, D_SHARED), D ** -0.5),
        "moe_ws3": nrm((DEPTH, D, D_SHARED), D ** -0.5),
        "moe_ws2": nrm((DEPTH, D_SHARED, D), D_SHARED ** -0.5 * DN_BETA),
    }


def reference(x, c, ctx, c_ctx, w_ada, b_ada, ln_mix_g, ln_mix_b, ln_ffn_g, ln_ffn_b,
              attn_w_qkv, attn_w_o, attn_sink,
              gmlp_w_in, gmlp_b_in, gmlp_v_g, gmlp_v_b, gmlp_w_s, gmlp_b_s, gmlp_w_o,
              moe_w_router, moe_bias, moe_w1, moe_w3, moe_w2, moe_ws1, moe_ws3, moe_ws2):
    B, S, D = x.shape
    C = ctx.shape[1]
    ROWS = S // GRID_W
    rope_cos, rope_sin = _axial_rope_tables(ROWS, x.dtype)
    silu_c = jax.nn.silu(c)
    silu_c_ctx = jax.nn.silu(c_ctx)
    last_ctx_reader = max(i for i in range(DEPTH) if i % N_MIXERS == MIXER_ATTN)
    h_ctx = ctx
    for i in range(DEPTH):
        mixer, j = i % N_MIXERS, i // N_MIXERS
        ctx_live = i <= last_ctx_reader
        update_ctx = i < last_ctx_reader
        sh_m, sc_m, g_m, sh_f, sc_f, g_f = jnp.split((silu_c @ w_ada[i] + b_ada[i])[:, None, :], 6, axis=-1)
        if ctx_live:
            csh_m, csc_m, cg_m, csh_f, csc_f, cg_f = jnp.split(silu_c_ctx @ w_ada[i] + b_ada[i], 6, axis=-1)
            hc_in = h_ctx * (1.0 + csc_m) + csh_m
        h_in = x * (1.0 + sc_m) + sh_m
        if mixer == MIXER_ATTN:
            y, y_c = _windowed_gqa(h_in, hc_in, attn_w_qkv[j], attn_w_o[j], attn_sink[j],
                                   rope_cos, rope_sin, update_ctx)
        else:
            gp = (gmlp_w_in[j], gmlp_b_in[j], gmlp_v_g[j], gmlp_v_b[j], gmlp_w_s[j], gmlp_b_s[j], gmlp_w_o[j])
            y = _chunk_gmlp(h_in, *gp)
            y_c = _chunk_gmlp(hc_in, *gp) if update_ctx else None
        x = _layernorm(DN_ALPHA * x + g_m * y, ln_mix_g[i], ln_mix_b[i])
        if update_ctx:
            h_ctx = _layernorm(DN_ALPHA * h_ctx + cg_m * y_c, ln_mix_g[i], ln_mix_b[i])
        f_in = (x * (1.0 + sc_f) + sh_f).reshape(B * S, D)
        if update_ctx:
            fc_in = (h_ctx * (1.0 + csc_f) + csh_f).reshape(B * C, D)
            f_in = jnp.concatenate([f_in, fc_in], axis=0)
        f = _moe(f_in, moe_w_router[i], moe_bias[i], moe_w1[i], moe_w3[i], moe_w2[i],
                 moe_ws1[i], moe_ws3[i], moe_ws2[i])
        x = _layernorm(DN_ALPHA * x + g_f * f[:B * S].reshape(B, S, D), ln_ffn_g[i], ln_ffn_b[i])
        if update_ctx:
            h_ctx = _layernorm(DN_ALPHA * h_ctx + cg_f * f[B * S:].reshape(B, C, D), ln_ffn_g[i], ln_ffn_b[i])
    return x
```

```python
import contextlib
import math
import numpy as np
import concourse.bass as bass
import concourse.mybir as mybir
from concourse.bass_utils import run_bass_kernel_spmd

F32 = mybir.dt.float32
BF16 = mybir.dt.bfloat16
AF = mybir.ActivationFunctionType
ALU = mybir.AluOpType
AX = mybir.AxisListType

NCORES = 8
D = 2048
SEQ = 2048
CTX = 256
TT = 512
KC = D // 128
NTILE = 2 * SEQ // TT
NE = 64
DE = 512
ALPHA = (2 * 2) ** 0.25
LN_EPS = 1e-5
ENGS = ["tensor", "vector", "scalar", "gpsimd", "sync"]

GROUP_ROWS = {"A": 5856, "B": 8192, "C": 8192, "E": 8192}
WINFO = {
    "wada": ("A", 0, (2 * 2048, 12288)),
    "wqkv": ("A", 3072, (2048, 2560)),
    "wqksw": ("A", 3392, (2048, 2304)),
    "wo": ("A", 3680, (2048, 2048)),
    "win": ("A", 3936, (2048, 8192)),
    "wout": ("A", 4960, (4096, 2048)),
    "ws1": ("A", 5472, (2 * 2048, 512)),
    "ws3": ("A", 5600, (2 * 2048, 512)),
    "ws2": ("A", 5728, (2 * 512, 2048)),
    "w1": ("B", 0, (2 * 64 * 2048, 512)),
    "w3": ("C", 0, (2 * 64 * 2048, 512)),
    "w2": ("E", 0, (2 * 64 * 512, 2048)),
}


class Tok:
    __slots__ = ("key", "val")

    def __init__(self, key, val):
        self.key = key
        self.val = val


class Res:
    __slots__ = ("name", "w", "rs")

    def __init__(self, name):
        self.name = name
        self.w = None
        self.rs = []


class Prog:
    def __init__(self, nc):
        self.nc = nc
        self.lists = {e: [] for e in ENGS}
        self.sem_names = []
        self.sems = {}
        self.count = {}
        self.waited = {e: {} for e in ENGS}
        self.events = {e: [] for e in ENGS}
        for e in ENGS:
            self.new_sem("p_" + e)

    def new_sem(self, key):
        self.sem_names.append(key)
        self.count[key] = 0
        return key

    def _wait(self, eng, tok):
        if tok is None:
            return
        if eng == "tensor" and tok.key == "p_tensor":
            return
        w = self.waited[eng]
        if w.get(tok.key, 0) >= tok.val:
            return
        w[tok.key] = tok.val
        key, val = tok.key, tok.val
        self.events[eng].append(("w", key, val))
        self.lists[eng].append(lambda e, key=key, val=val: e.wait_ge(self.sems[key], val))

    def _deps(self, eng, reads, writes, extra):
        for r in reads:
            self._wait(eng, r.w)
        for wr in writes:
            self._wait(eng, wr.w)
            for t in wr.rs:
                self._wait(eng, t)
        for t in extra:
            self._wait(eng, t)

    def _mark(self, tok, reads, writes):
        for r in reads:
            r.rs.append(tok)
        for wr in writes:
            wr.w = tok
            wr.rs = []

    def op(self, eng, fn, reads=(), writes=(), extra=(), inc=True):
        pkey = "p_" + eng
        self._deps(eng, reads, writes, extra)
        if inc:
            self.count[pkey] += 1
            tok = Tok(pkey, self.count[pkey])
            self.events[eng].append(("i", pkey, 1))
            self.lists[eng].append(lambda e, fn=fn, k=pkey: fn(e).then_inc(self.sems[k], 1))
        else:
            tok = Tok(pkey, self.count[pkey] + 1)
            self.lists[eng].append(lambda e, fn=fn: fn(e))
        self._mark(tok, reads, writes)
        return tok

    def dma(self, eng, fn, sem, reads=(), writes=(), extra=()):
        self._deps(eng, reads, writes, extra)
        self.count[sem] += 16
        tok = Tok(sem, self.count[sem])
        self.events[eng].append(("i", sem, 16))
        self.lists[eng].append(lambda e, fn=fn, k=sem: fn(e).then_inc(self.sems[k], 16))
        self._mark(tok, reads, writes)
        return tok

    def cc(self, fn, sem, reads=(), writes=()):
        self._deps("gpsimd", reads, writes, ())
        self.count[sem] += 1
        tok = Tok(sem, self.count[sem])
        self.events["gpsimd"].append(("i", sem, 1))
        self.lists["gpsimd"].append(lambda e, fn=fn, k=sem: fn(e).then_inc(self.sems[k], 1))
        self._mark(tok, reads, writes)
        return tok

    def simulate(self):
        cnt = {k: 0 for k in self.sem_names}
        pos = {e: 0 for e in ENGS}
        progress = True
        while progress:
            progress = False
            for e in ENGS:
                ev = self.events[e]
                while pos[e] < len(ev):
                    kind, key, val = ev[pos[e]]
                    if kind == "w":
                        if cnt[key] < val:
                            break
                    else:
                        cnt[key] += val
                    pos[e] += 1
                    progress = True
        stuck = {e: (pos[e], len(self.events[e]), self.events[e][pos[e]], cnt[self.events[e][pos[e]][1]]) for e in ENGS if pos[e] < len(self.events[e])}
        return stuck

    def finalize(self, final_waits=()):
        nc = self.nc
        with contextlib.ExitStack() as st:
            for k in self.sem_names:
                self.sems[k] = st.enter_context(nc.semaphore(k))
            for t in final_waits:
                self._wait("sync", t)
            stuck = self.simulate()
            assert not stuck, f"DEADLOCK in semaphore program: {stuck}"
            block = st.enter_context(nc.Block())
            lists = self.lists

            @block.tensor
            def _(e):
                for f in lists["tensor"]:
                    f(e)

            @block.vector
            def _(e):
                for f in lists["vector"]:
                    f(e)

            @block.scalar
            def _(e):
                for f in lists["scalar"]:
                    f(e)

            @block.gpsimd
            def _(e):
                for f in lists["gpsimd"]:
                    f(e)

            @block.sync
            def _(e):
                for f in lists["sync"]:
                    f(e)


def build(tiles=None, dbg=False, sim=False, garbage=False, stop_after=None):
    if tiles is None:
        tiles = list(range(NTILE))
    nc = bass.Bass("TRN2", target_bir_lowering=False)
    P = Prog(nc)
    st = contextlib.ExitStack()

    def din(name, shape, dt=F32):
        return nc.dram_tensor(name, list(shape), dt, kind="ExternalInput")

    xT_h = din("xT", [D, 2 * SEQ])
    ctxT_h = din("ctxT", [D, 2 * CTX])
    cT_h = din("cT", [D, 3])
    badaT_h = din("badaT", [128, 2 * 96])
    lnp_h = din("lnp", [128, 8 * KC])
    wr_h = din("wr", [2 * D, NE])
    rbias_h = din("rbias", [2, NE])
    sink_h = din("sink", [1, 32])
    binu_h = din("binu", [128, 32])
    binv_h = din("binv", [1, 4096])
    vgb_h = din("vgb", [128, 64])
    wsp_h = din("wsp", [128, 8 * 128])
    bsp_h = din("bsp", [1, 8 * 128])
    ropeC_h = din("ropeC", [64, SEQ])
    ropeS_h = din("ropeS", [64, SEQ])
    masks_h = din("masks", [128, 256])
    ident_h = din("ident", [128, 128])
    shard_h = {g: (nc.dram_tensor("shard" + g, [GROUP_ROWS[g], 2048], F32) if garbage else din("shard" + g, [GROUP_ROWS[g], 2048])) for g in "ABCE"}
    outT_h = nc.dram_tensor("outT", [D, 2 * SEQ], F32, kind="ExternalOutput")
    if dbg:
        dbg_h = nc.dram_tensor("dbg", [len(tiles) * 4, D, TT], F32, kind="ExternalOutput")
        dbgg_h = nc.dram_tensor("dbgg", [len(tiles) * 2, TT, 65], F32, kind="ExternalOutput")

    cast_h = {g: nc.dram_tensor("cast" + g, [GROUP_ROWS[g], 2048], BF16) for g in "ABCE"}
    gath_h = {g: nc.dram_tensor("gath" + g, [8 * GROUP_ROWS[g], 2048], BF16, addr_space="Local") for g in "ABCE"}
    kT_d = nc.dram_tensor("kT_d", [2, 4, 64, CTX + SEQ], BF16)
    v_d = nc.dram_tensor("v_d", [2, CTX + SEQ, 512], BF16)
    r_gath = {g: Res("gath" + g) for g in "ABCE"}
    r_cast = {g: Res("cast" + g) for g in "ABCE"}
    r_kvd = Res("kvd")

    def AP(h, off, dims):
        return bass.AP(h, off, [list(d) for d in dims])

    def sb(name, shape, dt):
        return st.enter_context(nc.sbuf_tensor(name, list(shape), dt))

    xT = sb("xT_s", [128, KC, TT], F32)
    hT = sb("hT_s", [128, KC, TT], BF16)
    ARENA = 120 * 1024
    arena = sb("arena", [128, ARENA], mybir.dt.uint8)
    ident = sb("ident_s", [128, 128], F32)
    ones32 = sb("ones32", [128, 128], F32)
    ones16 = sb("ones16", [128, 128], BF16)
    masks = sb("masks_s", [128, 2, 128], BF16)
    modT = sb("modT", [128, 2, 96, 3], F32)
    lnp = sb("lnp_s", [128, 4, 2, KC], F32)
    scal = sb("scal", [128, 12, 3, KC], F32)
    wr_s = sb("wr_s", [128, 2, KC, NE], F32)
    rbias_s = sb("rbias_s", [128, 2, NE], F32)
    sinkexp = sb("sinkexp", [128, 32], F32)
    binu = sb("binu_s", [128, 32], F32)
    vgb = sb("vgb_s", [128, 64], F32)
    gates = sb("gates", [128, 4, 65], F32)
    rt = sb("rt", [128, 4, 224], F32)
    small = sb("small", [128, 64], F32)
    epsc = sb("epsc", [128, 2], F32)
    psum = st.enter_context(nc.psum_tensor("psum", [128, 8, 512], F32))

    r_xT, r_hT = Res("xT"), Res("hT")
    r_ps = [Res(f"ps{i}") for i in range(8)]
    r_const = Res("const")
    r_mod = Res("mod")
    r_scal = Res("scal")
    r_gates = Res("gates")
    r_rt = Res("rt")
    r_stat = [Res(f"stat{i}") for i in range(8)]
    r_small = Res("small")

    def carve(off, shape, dt):
        nbytes = int(np.prod(shape)) * (2 if dt == BF16 else 4)
        assert off + nbytes <= ARENA, (off, nbytes)
        v = arena[:, off:off + nbytes].bitcast(dt)
        if len(shape) == 1:
            return v
        names = " ".join(f"a{i}" for i in range(len(shape)))
        kw = {f"a{i}": int(shape[i]) for i in range(1, len(shape))}
        return v.rearrange(f"p ({names}) -> p {names}", **kw)

    K = 1024
    w13 = [carve(i * 32 * K, [2, KC, DE], BF16) for i in range(2)]
    w2s = carve(64 * K, [4, D], BF16)
    acc = carve(80 * K, [4, D], F32)
    aT = carve(112 * K, [4, TT], BF16)
    s1 = [carve(116 * K + i * 2 * K, [TT], F32) for i in range(2)]
    fin32 = [carve(112 * K + i * 2 * K, [TT], F32) for i in range(2)]
    stat = carve(0, [8, TT], F32)
    r_w13 = [Res("w13_0"), Res("w13_1")]
    r_w2s, r_acc, r_aT = Res("w2s"), Res("acc"), Res("aT")
    r_s1 = [Res("s1_0"), Res("s1_1")]
    r_fin = [Res("fin0"), Res("fin1")]
    qrot = carve(0, [32, TT], BF16)
    oT = carve(32 * K, [KC, TT], BF16)
    kTs = carve(48 * K, [4, CTX + 768], BF16)
    vS = carve(56 * K, [8, 512], BF16)
    pT = [carve(64 * K + i * K, [TT], BF16) for i in range(4)]
    den = [carve(68 * K + i * 2 * K, [TT], F32) for i in range(2)]
    ropeC = carve(72 * K, [TT], F32)
    ropeS = carve(74 * K, [TT], F32)
    rtmp = [carve(76 * K + i * 2 * K, [TT], F32) for i in range(4)]
    wq = [carve(84 * K + i * 8 * K, [KC, 256], BF16) for i in range(4)]
    r_qrot, r_oT, r_kTs, r_vS = Res("qrot"), Res("oT"), Res("kTs"), Res("vS")
    r_pT = [Res(f"pT{i}") for i in range(4)]
    r_den = [Res("den0"), Res("den1")]
    r_rope = Res("rope")
    r_rtmp = [Res(f"rtmp{i}") for i in range(4)]
    r_wq = [Res(f"wq{i}") for i in range(4)]
    wk = carve(0, [KC, 256], BF16)
    wksw = carve(8 * K, [KC, 256], BF16)
    wv2 = carve(16 * K, [KC, 512], BF16)
    kout = [carve(32 * K + i * K, [TT], BF16) for i in range(2)]
    vout = [carve(34 * K + i * K, [512], BF16) for i in range(2)]
    r_wkv = Res("wkv")
    r_kout = [Res("kout0"), Res("kout1")]
    r_vout = [Res("vout0"), Res("vout1")]
    wadab = [carve(i * 48 * K, [KC, 1536], BF16) for i in range(2)]
    siluc = carve(96 * K, [KC, 3], BF16)
    craw = carve(97 * K, [KC, 3], F32)
    badaT = carve(98 * K, [2, 96], F32)
    wsp32 = carve(100 * K, [8, 128], F32)
    wsp16 = carve(104 * K, [8, 128], BF16)
    r_wadab = [Res("wada0"), Res("wada1")]
    r_siluc = Res("siluc")
    uT = carve(0, [32, TT], BF16)
    vg32 = carve(32 * K, [4096], F32)
    vln = [carve(48 * K + i * 8 * K, [4096], BF16) for i in range(2)]
    winb = [carve(64 * K + i * 16 * K, [KC, 512], BF16) for i in range(2)]
    woutb = [carve(96 * K + i * 8 * K, [32, 128], BF16) for i in range(2)]
    binvb = [carve(112 * K + i * K, [512], BF16) for i in range(2)]
    gtmp = [carve(114 * K + i * K, [128], F32) for i in range(4)]
    bnst = carve(118 * K, [8, 6], F32)
    r_uT, r_vg32 = Res("uT"), Res("vg32")
    r_vln = [Res("vln0"), Res("vln1")]
    r_winb = [Res("winb0"), Res("winb1")]
    r_woutb = [Res("woutb0"), Res("woutb1")]
    r_binvb = [Res("binvb0"), Res("binvb1")]
    r_gtmp = [Res(f"gtmp{i}") for i in range(4)]
    r_bnst = Res("bnst")
    wsT = sb("wsT", [128, 8, 128], BF16)
    Bt = sb("Bt", [128, 32, 128], F32)
    bsbc = carve(106 * K, [8, 128], F32)
    r_arena = Res("arena")

    s_misc = P.new_sem("misc")
    s_cast = {g: P.new_sem("cast" + g) for g in "ABCE"}
    s_cc = {g: P.new_sem("cc" + g) for g in "ABCE"}
    s_rope = P.new_sem("rope")
    s_mask = P.new_sem("mask")
    s_b = [P.new_sem("binv0"), P.new_sem("binv1")]
    s_kt = P.new_sem("ktr")
    s_v = P.new_sem("vr")
    s_x = P.new_sem("xload")
    s_w = [P.new_sem(f"wslot{i}") for i in range(6)]
    s_kvw = P.new_sem("kvw")
    s_kvr = P.new_sem("kvr")
    s_out = P.new_sem("out")
    s_dbg = P.new_sem("dbg")

    arena_res = (r_w13 + [r_w2s, r_acc, r_aT] + r_s1 + r_fin + [r_qrot, r_oT, r_kTs, r_vS] + r_pT + r_den + [r_rope] + r_rtmp + r_wq
                 + [r_wkv] + r_kout + r_vout + r_wadab + [r_siluc, r_uT, r_vg32] + r_vln + r_winb + r_woutb + r_binvb + r_gtmp + [r_bnst] + r_stat)

    def phase_switch():
        best = {}
        for r in arena_res:
            for t in ([r.w] if r.w is not None else []) + r.rs:
                if best.get(t.key, 0) < t.val:
                    best[t.key] = t.val
        toks = [Tok(k, v) for k, v in best.items()]
        for r in arena_res:
            r.w = None
            r.rs = list(toks)

    def act(out, in_, func, reads, writes, bias=None, scale=None):
        kw = {}
        if bias is not None:
            kw["bias"] = bias
        if scale is not None:
            kw["scale"] = scale
        return P.op("scalar", lambda e: e.activation(out=out, in_=in_, func=func, **kw), reads=reads, writes=writes)

    def vts(out, in0, s1_, s2_, op0, op1, reads, writes, eng="vector"):
        if op1 is None:
            return P.op(eng, lambda e: getattr(e, "tensor_scalar")(out=out, in0=in0, scalar1=s1_, scalar2=None, op0=op0), reads=reads, writes=writes)
        return P.op(eng, lambda e: getattr(e, "tensor_scalar")(out=out, in0=in0, scalar1=s1_, scalar2=s2_, op0=op0, op1=op1), reads=reads, writes=writes)

    def vtt(out, in0, in1, op, reads, writes, eng="vector"):
        return P.op(eng, lambda e: e.tensor_tensor(out=out, in0=in0, in1=in1, op=op), reads=reads, writes=writes)

    def vstt(out, in0, scalar, in1, op0, op1, reads, writes):
        return P.op("vector", lambda e: e.scalar_tensor_tensor(out=out, in0=in0, scalar=scalar, in1=in1, op0=op0, op1=op1), reads=reads, writes=writes)

    def vcopy(out, in_, reads, writes, eng="vector"):
        return P.op(eng, lambda e: e.tensor_copy(out=out, in_=in_), reads=reads, writes=writes)

    def mm(out, lhsT, rhs, start, stop, reads, writes, inc=None):
        return P.op("tensor", lambda e: e.matmul(out, lhsT=lhsT, rhs=rhs, start=start, stop=stop),
                    reads=reads, writes=writes, inc=(stop if inc is None else inc))

    def wload(dst, name, row0, nrows, col0, ncols, res, sem, queue="sync", extra_reads=()):
        g, off, (R, C) = WINFO[name]
        rpr = R // 8
        toks = []
        r0 = row0
        while r0 < row0 + nrows:
            rank = r0 // rpr
            r1 = min(row0 + nrows, (rank + 1) * rpr)
            base = (rank * GROUP_ROWS[g] + off) * 2048 + (r0 - rank * rpr) * C + col0
            nk = (r1 - r0) // 128
            k0 = (r0 - row0) // 128
            src = AP(gath_h[g], base, [[C, 128], [128 * C, nk], [1, ncols]])
            d = dst[:, k0:k0 + nk, :]
            toks.append(P.dma(queue, lambda e, d=d, src=src: e.dma_start(out=d, in_=src), sem,
                              reads=[r_gath[g]] + list(extra_reads), writes=[res]))
            r0 = r1
        return toks

    for g in "ABCE":
        R = GROUP_ROWS[g]
        CH = 2048
        for i in range(0, R, CH):
            n = min(CH, R - i)
            src = shard_h[g].ap()[i:i + n, :]
            dst = cast_h[g].ap()[i:i + n, :]
            P.dma("gpsimd", lambda e, src=src, dst=dst: e.dma_start(out=dst, in_=src), s_cast[g], writes=[r_cast[g]])
        cin, cout = cast_h[g].ap(), gath_h[g].ap()
        if sim:
            for rr in range(NCORES):
                dsts = cout[rr * R:(rr + 1) * R, :]
                P.dma("gpsimd", lambda e, cin=cin, dsts=dsts: e.dma_start(out=dsts, in_=cin), s_cast[g], reads=[r_cast[g]], writes=[r_gath[g]])
            continue
        cct = P.cc(lambda e, cin=cin, cout=cout: e.collective_compute("AllGather", ALU.bypass, replica_groups=[list(range(NCORES))],
                                                                      ins=[cin], outs=[cout]),
                   s_cc[g], reads=[r_cast[g]], writes=[r_gath[g]])
        P._wait("gpsimd", cct)

    def ld(dst, src, q="sync", writes=(r_const,)):
        return P.dma(q, lambda e: e.dma_start(out=dst, in_=src), s_misc, writes=list(writes))

    ld(ident[:], ident_h.ap())
    ld(lnp[:], lnp_h.ap().rearrange("p (a l k) -> p a l k", a=4, l=2))
    ld(wr_s[:], wr_h.ap().rearrange("(l k p) e -> p l k e", l=2, p=128))
    ld(rbias_s[:], rbias_h.ap().rearrange("l e -> (l e)").partition_broadcast(128).rearrange("p (l e) -> p l e", l=2))
    ld(sinkexp[:], sink_h.ap().rearrange("o h -> (o h)").partition_broadcast(128))
    ld(binu[:], binu_h.ap())
    ld(vgb[:], vgb_h.ap())
    ld(bsbc[:], bsp_h.ap().rearrange("o f -> (o f)").partition_broadcast(128).rearrange("p (g q) -> p g q", g=8))
    ld(craw, cT_h.ap().rearrange("(k p) b -> p k b", p=128), writes=[r_const, r_siluc])
    ld(badaT, badaT_h.ap().rearrange("p (l n) -> p l n", l=2), writes=[r_const, r_siluc])
    ld(wsp32, wsp_h.ap().rearrange("p (g q) -> p g q", g=8), writes=[r_const, r_siluc])
    P.dma("gpsimd", lambda e: e.dma_start(out=masks[:], in_=masks_h.ap().rearrange("p (a q) -> p a q", a=2)), s_mask, writes=[r_const])
    P.op("vector", lambda e: e.memset(ones32[:], 1.0), writes=[r_const])
    P.op("vector", lambda e: e.memset(epsc[:], LN_EPS), writes=[r_const])
    P.op("vector", lambda e: e.memset(ones16[:], 1.0), writes=[r_const])
    act(sinkexp[:], sinkexp[:], AF.Exp, [r_const], [r_const])
    act(siluc, craw, AF.Silu, [r_siluc], [r_siluc])
    vcopy(wsp16, wsp32, [r_siluc], [r_siluc])
    pst = psum[:].rearrange("p a f -> p (a f)").bitcast(BF16)
    identb = sb("identb", [128, 128], BF16)
    vcopy(identb[:], ident[:], [r_const], [r_const])
    for g8 in range(8):
        P.op("tensor", lambda e, g8=g8: e.transpose(pst[:, g8 * 128:(g8 + 1) * 128], wsp16[:, g8, :], identb[:]),
             reads=[r_siluc, r_const], writes=[r_ps[0]], inc=(g8 == 7))
    vcopy(wsT[:].rearrange("q g p -> q (g p)"), pst[:, 0:1024], [r_ps[0]], [r_const])
    for g8 in range(8):
        mm(psum[:, 1 + g8 // 4, (g8 % 4) * 128:(g8 % 4 + 1) * 128],
           ones16[:], wsT[:, g8, :], True, True, [r_const], [r_ps[1 + g8 // 4]])
    for fc in range(32):
        g8 = fc // 4
        vstt(Bt[:, fc, :], psum[:, 1 + g8 // 4, (g8 % 4) * 128:(g8 % 4 + 1) * 128], vgb[:, 32 + fc:33 + fc], bsbc[:, g8, :],
             ALU.mult, ALU.add, [r_ps[1], r_ps[2], r_const], [r_const])

    for l in range(2):
        bank = 3 + l
        first = True
        for nb in range(8):
            slot = (l * 8 + nb) % 2
            wload(wadab[slot], "wada", l * 2048, 2048, nb * 1536, 1536, r_wadab[slot], s_w[slot])
            for j in range(12):
                n = nb * 12 + j
                for kc in range(KC):
                    P.op("tensor", lambda e, slot=slot, j=j, kc=kc, n=n, bank=bank: e.matmul(
                        psum[:, bank, n * 3:(n + 1) * 3], lhsT=wadab[slot][:, kc, j * 128:(j + 1) * 128], rhs=siluc[:, kc, :],
                        start=(kc == 0), stop=(kc == KC - 1)),
                        reads=[r_wadab[slot], r_siluc], writes=[r_ps[bank]], inc=(kc == KC - 1))
                    first = False
        vtt(modT[:, l, :, :], psum[:, bank, 0:288].rearrange("p (n b) -> p n b", b=3),
            badaT[:, l, :].unsqueeze(2).broadcast_to([128, 96, 3]), ALU.add, [r_ps[bank], r_siluc], [r_mod])

    def modv(l, part):
        return modT[:, l, part * KC:(part + 1) * KC, :].rearrange("p k b -> p b k")

    def lnv(a, l):
        return lnp[:, a, l, :].unsqueeze(1).broadcast_to([128, 3, KC])
    SH_M, SC_M, G_M, SH_F, SC_F, G_F = range(6)
    tmpA = rt[:, 0, 0:48].rearrange("p (b k) -> p b k", b=3)
    vts(scal[:, 0], modv(0, SC_M), 1.0, None, ALU.add, None, [r_mod], [r_scal])
    vcopy(scal[:, 1], modv(0, SH_M), [r_mod], [r_scal])
    vcopy(scal[:, 2], modv(0, G_M), [r_mod], [r_scal])
    vcopy(scal[:, 5], modv(0, G_F), [r_mod], [r_scal])
    vcopy(scal[:, 8], modv(1, G_M), [r_mod], [r_scal])
    vcopy(scal[:, 11], modv(1, G_F), [r_mod], [r_scal])
    for (ia, ib, lsc, psc, psh, ag, ab, ll) in [(3, 4, 0, SC_F, SH_F, 0, 1, 0), (6, 7, 1, SC_M, SH_M, 2, 3, 0), (9, 10, 1, SC_F, SH_F, 0, 1, 1)]:
        vts(tmpA, modv(lsc, psc), 1.0, None, ALU.add, None, [r_mod], [r_rt])
        vtt(scal[:, ia], tmpA, lnv(ag, ll), ALU.mult, [r_rt, r_const], [r_scal])
        vtt(scal[:, ib], tmpA, lnv(ab, ll), ALU.mult, [r_rt, r_const], [r_scal])
        vtt(scal[:, ib], scal[:, ib], modv(lsc, psh), ALU.add, [r_mod], [r_scal])

    def load_xT(src_h, t0):
        src = src_h.ap()[:, t0:t0 + TT].rearrange("(k p) t -> p k t", p=128)
        P.dma("sync", lambda e: e.dma_start(out=xT[:], in_=src), s_x, writes=[r_xT])

    def modulate(idx_scale, idx_bias, b, src=None):
        for kc in range(KC):
            eng = "vector" if kc % 2 == 0 else "gpsimd"
            vts(hT[:, kc, :], xT[:, kc, :], scal[:, idx_scale, b, kc:kc + 1], scal[:, idx_bias, b, kc:kc + 1],
                ALU.mult, ALU.add, [r_xT, r_scal], [r_hT], eng=eng)

    phase_switch()
    wload(wk, "wqkv", 0, 2048, 2048, 256, r_wkv, s_w[2])
    wload(wksw, "wqksw", 0, 2048, 2048, 256, r_wkv, s_w[2])
    for kvh in range(4):
        for dup in range(2):
            g_, off_, (R_, C_) = WINFO["wqkv"]
            for rank in range(8):
                base = (rank * GROUP_ROWS[g_] + off_) * 2048 + 2304 + kvh * 64
                src = AP(gath_h[g_], base, [[C_, 128], [128 * C_, 2], [1, 64]])
                d = wv2[:, rank * 2:rank * 2 + 2, (kvh * 2 + dup) * 64:(kvh * 2 + dup + 1) * 64]
                P.dma("sync", lambda e, d=d, src=src: e.dma_start(out=d, in_=src), s_w[2], reads=[r_gath[g_]], writes=[r_wkv])

    kv_tiles = [("ctx", 0)] + [("x", t) for t in range(NTILE)]
    ko_i = 0
    vo_i = 0
    for kind, t in kv_tiles:
        if kind == "ctx":
            load_xT(ctxT_h, 0)
            modulate(0, 1, 2)
        else:
            load_xT(xT_h, t * TT)
            modulate(0, 1, t // 4)
        s_i, ti = t // 4, t % 4
        if kind == "x":
            tp = ti * TT
            P.dma("sync", lambda e, tp=tp: e.dma_start(out=ropeC[0:64], in_=ropeC_h.ap()[:, tp:tp + TT]), s_rope, writes=[r_rope])
            P.dma("sync", lambda e, tp=tp: e.dma_start(out=ropeS[0:64], in_=ropeS_h.ap()[:, tp:tp + TT]), s_rope, writes=[r_rope])
        for kvh in range(4):
            for kc in range(KC):
                mm(psum[0:64, 0, :], wk[:, kc, kvh * 64:(kvh + 1) * 64], hT[:, kc, :], kc == 0, kc == KC - 1,
                   [r_wkv, r_hT], [r_ps[0]])
            ko = ko_i % 2
            ko_i += 1
            if kind == "x":
                for kc in range(KC):
                    mm(psum[0:64, 1, :], wksw[:, kc, kvh * 64:(kvh + 1) * 64], hT[:, kc, :], kc == 0, kc == KC - 1,
                       [r_wkv, r_hT], [r_ps[1]])
                vtt(rtmp[0][0:64], psum[0:64, 1, :], ropeS[0:64], ALU.mult, [r_ps[1], r_rope], [r_rtmp[0]])
                vtt(rtmp[1][0:64], psum[0:64, 0, :], ropeC[0:64], ALU.mult, [r_ps[0], r_rope], [r_rtmp[1]])
                vtt(kout[ko][0:64], rtmp[0][0:64], rtmp[1][0:64], ALU.add, [r_rtmp[0], r_rtmp[1]], [r_kout[ko]], eng="gpsimd")
                dst = kT_d.ap()[s_i, kvh, :, CTX + ti * TT:CTX + (ti + 1) * TT]
                P.dma("gpsimd", lambda e, dst=dst, ko=ko: e.dma_start(out=dst, in_=kout[ko][0:64]), s_kvw, reads=[r_kout[ko]], writes=[r_kvd])
            else:
                vcopy(kout[ko][0:64], psum[0:64, 0, :], [r_ps[0]], [r_kout[ko]])
                for s2 in range(2):
                    dst = kT_d.ap()[s2, kvh, :, 0:CTX]
                    P.dma("gpsimd", lambda e, dst=dst, ko=ko, s2=s2: e.dma_start(out=dst, in_=kout[ko][0:64, s2 * CTX:(s2 + 1) * CTX]),
                          s_kvw, reads=[r_kout[ko]], writes=[r_kvd])
        for g in range(4):
            bank = 2 + g % 2
            for kc in range(KC):
                mm(psum[:, bank, :], hT[:, kc, g * 128:(g + 1) * 128], wv2[:, kc, :], kc == 0, kc == KC - 1,
                   [r_wkv, r_hT], [r_ps[bank]])
            vo = vo_i % 2
            vo_i += 1
            act(vout[vo], psum[:, bank, :], AF.Copy, [r_ps[bank]], [r_vout[vo]])
            if kind == "x":
                dst = v_d.ap()[s_i, CTX + ti * TT + g * 128:CTX + ti * TT + (g + 1) * 128, :]
            else:
                dst = v_d.ap()[g // 2, (g % 2) * 128:(g % 2 + 1) * 128, :]
            P.dma("gpsimd", lambda e, dst=dst, vo=vo: e.dma_start(out=dst, in_=vout[vo]), s_kvw, reads=[r_vout[vo]], writes=[r_kvd])

    def layernorm(l, which, b, nxt):
        ga, gb = (0, 1) if which == "mix" else (2, 3)
        mean, msq, var, nmr = stat[:, 0, :], stat[:, 1, :], stat[:, 2, :], stat[:, 3, :]
        for kc in range(KC):
            sq = stat[:, 4 + kc % 2, :]
            act(sq, xT[:, kc, :], AF.Square, [r_xT], [r_stat[4 + kc % 2]])
            mm(psum[:, 0, :], ones32[:], xT[:, kc, :], kc == 0, kc == KC - 1, [r_xT, r_const], [r_ps[0]])
            mm(psum[:, 1, :], ones32[:], sq, kc == 0, kc == KC - 1, [r_stat[4 + kc % 2]], [r_ps[1]], inc=True)
        vts(mean, psum[:, 0, :], 1.0 / D, None, ALU.mult, None, [r_ps[0]], [r_stat[0]])
        vtt(msq, mean, mean, ALU.mult, [r_stat[0]], [r_stat[1]])
        vstt(var, psum[:, 1, :], 1.0 / D, msq, ALU.mult, ALU.subtract, [r_ps[1], r_stat[1]], [r_stat[2]])
        act(var, var, AF.Sqrt, [r_stat[2], r_const], [r_stat[2]], bias=epsc[:, 0:1])
        P.op("vector", lambda e: e.reciprocal(out=var, in_=var), reads=[r_stat[2]], writes=[r_stat[2]])
        vstt(nmr, mean, -1.0, var, ALU.mult, ALU.mult, [r_stat[0], r_stat[2]], [r_stat[3]])
        for kc in range(KC):
            t = stat[:, 6 + kc % 2, :]
            rr = r_stat[6 + kc % 2]
            vtt(t, xT[:, kc, :], var, ALU.mult, [r_xT, r_stat[2]], [rr])
            vtt(t, t, nmr, ALU.add, [rr, r_stat[3]], [rr], eng="gpsimd")
            act(xT[:, kc, :], t, AF.Identity, [rr, r_const], [r_xT], bias=lnp[:, gb, l, kc:kc + 1], scale=lnp[:, ga, l, kc:kc + 1])
            if nxt is not None:
                act(hT[:, kc, :], t, AF.Identity, [rr, r_scal], [r_hT], bias=scal[:, nxt[1], b, kc:kc + 1], scale=scal[:, nxt[0], b, kc:kc + 1])

    def dump(idx):
        if not dbg:
            return
        dst = dbg_h.ap()[idx].rearrange("(k p) t -> p k t", p=128)
        P.dma("gpsimd", lambda e: e.dma_start(out=dst, in_=xT[:]), s_dbg, reads=[r_xT])

    def attention(tile):
        s_i, ti = tile // 4, tile % 4
        b = s_i
        B0 = ti * 4
        wlo = max(B0 - 1, 0)
        whi = min(B0 + 4, 15)
        nwb = whi - wlo + 1
        woff = wlo - (B0 - 1)
        src = kT_d.ap()[s_i, :, :, 0:CTX].rearrange("k d t -> d k t")
        P.dma("sync", lambda e: e.dma_start(out=kTs[0:64, :, 0:CTX], in_=src), s_kt, reads=[r_kvd], writes=[r_kTs])
        src2 = kT_d.ap()[s_i, :, :, CTX + wlo * 128:CTX + (whi + 1) * 128].rearrange("k d t -> d k t")
        P.dma("sync", lambda e: e.dma_start(out=kTs[0:64, :, CTX + woff * 128:CTX + (woff + nwb) * 128], in_=src2), s_kt, reads=[r_kvd], writes=[r_kTs])
        src3 = v_d.ap()[s_i, 0:CTX, :].rearrange("(c p) f -> p c f", p=128)
        P.dma("sync", lambda e: e.dma_start(out=vS[:, 0:2, :], in_=src3), s_v, reads=[r_kvd], writes=[r_vS])
        src4 = v_d.ap()[s_i, CTX + wlo * 128:CTX + (whi + 1) * 128, :].rearrange("(c p) f -> p c f", p=128)
        P.dma("sync", lambda e: e.dma_start(out=vS[:, 2 + woff:2 + woff + nwb, :], in_=src4), s_v, reads=[r_kvd], writes=[r_vS])
        tp = ti * TT
        P.dma("sync", lambda e: e.dma_start(out=ropeC[0:64], in_=ropeC_h.ap()[:, tp:tp + TT]), s_rope, writes=[r_rope])
        P.dma("sync", lambda e: e.dma_start(out=ropeS[0:64], in_=ropeS_h.ap()[:, tp:tp + TT]), s_rope, writes=[r_rope])
        for hg in range(8):
            sq_, ss_ = (hg % 2) * 2, (hg % 2) * 2 + 1
            wload(wq[sq_], "wqkv", 0, 2048, hg * 256, 256, r_wq[sq_], s_w[sq_])
            wload(wq[ss_], "wqksw", 0, 2048, hg * 256, 256, r_wq[ss_], s_w[ss_])
            for hh in range(4):
                h = hg * 4 + hh
                bq, bs_ = 6, 7
                for kc in range(KC):
                    mm(psum[0:64, bq, :], wq[sq_][:, kc, hh * 64:(hh + 1) * 64], hT[:, kc, :], kc == 0, kc == KC - 1,
                       [r_wq[sq_], r_hT], [r_ps[bq]])
                for kc in range(KC):
                    mm(psum[0:64, bs_, :], wq[ss_][:, kc, hh * 64:(hh + 1) * 64], hT[:, kc, :], kc == 0, kc == KC - 1,
                       [r_wq[ss_], r_hT], [r_ps[bs_]])
                i0, i1 = (h % 2) * 2, (h % 2) * 2 + 1
                vtt(rtmp[i0][0:64], psum[0:64, bs_, :], ropeS[0:64], ALU.mult, [r_ps[bs_], r_rope], [r_rtmp[i0]])
                vtt(rtmp[i1][0:64], psum[0:64, bq, :], ropeC[0:64], ALU.mult, [r_ps[bq], r_rope], [r_rtmp[i1]])
                vtt(qrot[0:64, h, :], rtmp[i0][0:64], rtmp[i1][0:64], ALU.add, [r_rtmp[i0], r_rtmp[i1]], [r_qrot], eng="gpsimd")
        pi = 0
        it = 0
        for kvh in range(4):
            for qb in range(4):
                gb_ = B0 + qb
                chunks = [("c", 0, None), ("c", 1, None)]
                if gb_ - 1 >= 0:
                    chunks.append(("w", qb, 0))
                chunks.append(("w", qb + 1, None))
                if gb_ + 1 <= 15:
                    chunks.append(("w", qb + 2, 1))
                for half in range(2):
                    rows = slice(half * 64, (half + 1) * 64)
                    bD, bO = 2 + it % 2, 4 + it % 2
                    it += 1
                    rhs_q = qrot[0:64, kvh * 8 + half:kvh * 8 + 8:2, qb * 128:(qb + 1) * 128]

                    def kcols(ch):
                        if ch[0] == "c":
                            return slice(ch[1] * 128, (ch[1] + 1) * 128)
                        return slice(CTX + ch[1] * 128, CTX + (ch[1] + 1) * 128)

                    def vidx(ch):
                        return ch[1] if ch[0] == "c" else 2 + ch[1]

                    def smm(ci):
                        ch = chunks[ci]
                        bS = ci % 2
                        mm(psum[:, bS, :].rearrange("p (m q) -> p m q", q=128), kTs[0:64, kvh, kcols(ch)], rhs_q, True, True, [r_kTs, r_qrot], [r_ps[bS]])
                    smm(0)
                    for ci, ch in enumerate(chunks):
                        if ci + 1 < len(chunks):
                            smm(ci + 1)
                        bS = ci % 2
                        p = pi % 4
                        pi += 1
                        act(pT[p], psum[:, bS, :], AF.Exp, [r_ps[bS]], [r_pT[p]], scale=0.125)
                        if ch[2] is not None:
                            pv = pT[p].rearrange("p (m q) -> p m q", q=128)
                            vtt(pv, pv, masks[:, ch[2], :].unsqueeze(1).broadcast_to([128, 4, 128]), ALU.mult, [r_pT[p], r_const], [r_pT[p]], eng="gpsimd")
                        first, last = ci == 0, ci == len(chunks) - 1
                        mm(psum[:, bD, :], ones16[:], pT[p], first, last, [r_pT[p], r_const], [r_ps[bD]])
                        mm(psum[:, bO, :], vS[:, vidx(ch), kvh * 128:(kvh + 1) * 128], pT[p], first, last, [r_pT[p], r_vS], [r_ps[bO]], inc=True)
                    dn = den[it % 2]
                    rd = r_den[it % 2]
                    dview = dn[rows].rearrange("p (m q) -> p m q", q=128)
                    se = sinkexp[rows, kvh * 8 + half:kvh * 8 + 8:2].unsqueeze(2).broadcast_to([64, 4, 128])
                    vtt(dview, psum[rows, bD, :].rearrange("p (m q) -> p m q", q=128), se, ALU.add, [r_ps[bD], r_const], [rd])
                    P.op("vector", lambda e, dn=dn, rows=rows: e.reciprocal(out=dn[rows], in_=dn[rows]), reads=[rd], writes=[rd])
                    vtt(oT[rows, kvh * 4:kvh * 4 + 4, qb * 128:(qb + 1) * 128], psum[rows, bO, :].rearrange("p (m q) -> p m q", q=128),
                        dview, ALU.mult, [r_ps[bO], rd], [r_oT])
        for kc in range(KC):
            vts(xT[:, kc, :], xT[:, kc, :], ALPHA, None, ALU.mult, None, [r_xT], [r_xT], eng="gpsimd")
        for dg in range(8):
            slot = dg % 2
            wload(wq[slot], "wo", 0, 2048, dg * 256, 256, r_wq[slot], s_w[slot])
            for dd in range(2):
                dc = dg * 2 + dd
                bank = 6 + dc % 2
                for pc in range(KC):
                    mm(psum[:, bank, :], wq[slot][:, pc, dd * 128:(dd + 1) * 128], oT[:, pc, :], pc == 0, pc == KC - 1,
                       [r_wq[slot], r_oT], [r_ps[bank]])
                vstt(xT[:, dc, :], psum[:, bank, :], scal[:, 2, b, dc:dc + 1], xT[:, dc, :], ALU.mult, ALU.add, [r_ps[bank], r_scal, r_xT], [r_xT])

    def moe(l, b, tile_slot):
        idxA, idxB = (3, 4) if l == 0 else (9, 10)
        gidx = 5 if l == 0 else 11
        for kc in range(KC):
            f = fin32[kc % 2]
            rf = r_fin[kc % 2]
            P.op("vector", lambda e, f=f, kc=kc: e.scalar_tensor_tensor(out=f, in0=xT[:, kc, :], scalar=modT[:, l, SC_F * KC + kc, b:b + 1],
                                                                       in1=xT[:, kc, :], op0=ALU.mult, op1=ALU.add),
                 reads=[r_xT, r_mod], writes=[rf])
            vts(f, f, modT[:, l, SH_F * KC + kc, b:b + 1], None, ALU.add, None, [rf, r_mod], [rf], eng="gpsimd")
            for g in range(4):
                mm(psum[:, g, 0:64], f[:, g * 128:(g + 1) * 128], wr_s[:, l, kc, :], kc == 0, kc == KC - 1,
                   [rf, r_const], [r_ps[g]], inc=(g == 3 or kc == KC - 1))
        sc = rt[:, :, 0:64]
        bsd = rt[:, :, 64:128]
        m8 = rt[:, :, 160:224].rearrange("p g (a k) -> p g a k", k=8)
        gs = rt[:, :, 128:136]
        g8s = rt[:, :, 136:144]
        pen = rt[:, :, 144:152]
        e8 = rt[:, :, 152:160]
        act(sc, psum[:, 0:4, 0:64], AF.Sigmoid, [r_ps[0], r_ps[1], r_ps[2], r_ps[3]], [r_rt])
        vtt(bsd, sc, rbias_s[:, l, :].unsqueeze(1).broadcast_to([128, 4, 64]), ALU.add, [r_rt, r_const], [r_rt])
        for g in range(4):
            for a in range(8):
                P.op("vector", lambda e, g=g, a=a: e.max(out=m8[:, g, a, :], in_=bsd[:, g, a * 8:(a + 1) * 8]), reads=[r_rt], writes=[r_rt])
        vtt(gs, m8[:, :, :, 0], m8[:, :, :, 1], ALU.add, [r_rt], [r_rt])
        for g in range(4):
            P.op("vector", lambda e, g=g: e.max(out=g8s[:, g, :], in_=gs[:, g, :]), reads=[r_rt], writes=[r_rt])
            vts(pen[:, g, :], gs[:, g, :], g8s[:, g, 3:4], 1e9, ALU.is_ge, ALU.mult, [r_rt], [r_rt])
        vts(pen, pen, -1e9, None, ALU.add, None, [r_rt], [r_rt])
        bsd4 = bsd.rearrange("p g (a k) -> p g a k", k=8)
        vtt(bsd4, bsd4, pen.unsqueeze(3).broadcast_to([128, 4, 8, 8]), ALU.add, [r_rt], [r_rt])
        for g in range(4):
            P.op("vector", lambda e, g=g: e.max(out=e8[:, g, :], in_=bsd[:, g, :]), reads=[r_rt], writes=[r_rt])
            vts(bsd[:, g, :], bsd[:, g, :], e8[:, g, 7:8], None, ALU.is_ge, None, [r_rt], [r_rt])
        vtt(sc, sc, bsd, ALU.mult, [r_rt], [r_rt])
        P.op("vector", lambda e: e.reduce_sum(out=small[:, 0:4], in_=sc, axis=AX.X), reads=[r_rt], writes=[r_small])
        P.op("vector", lambda e: e.reciprocal(out=small[:, 0:4], in_=small[:, 0:4]), reads=[r_small], writes=[r_small])
        for g in range(4):
            vts(gates[:, g, 0:64], sc[:, g, :], small[:, g:g + 1], 2.5, ALU.mult, ALU.mult, [r_rt, r_small], [r_gates])
        P.op("vector", lambda e: e.memset(gates[:, :, 64:65], 1.0), reads=[], writes=[r_gates])
        if dbg:
            dst = dbgg_h.ap()[tile_slot * 2 + l].rearrange("(g p) e -> p g e", p=128)
            P.dma("gpsimd", lambda e: e.dma_start(out=dst, in_=gates[:]), s_dbg, reads=[r_gates])
        for e_ in range(NE + 1):
            slot = e_ % 2
            if e_ < NE:
                wload(w13[slot][:, 0], "w1", (l * NE + e_) * D, D, 0, DE, r_w13[slot], s_w[slot])
                wload(w13[slot][:, 1], "w3", (l * NE + e_) * D, D, 0, DE, r_w13[slot], s_w[slot])
                wload(w2s, "w2", (l * NE + e_) * DE, DE, 0, D, r_w2s, s_w[2])
            else:
                wload(w13[slot][:, 0], "ws1", l * D, D, 0, DE, r_w13[slot], s_w[slot])
                wload(w13[slot][:, 1], "ws3", l * D, D, 0, DE, r_w13[slot], s_w[slot])
                wload(w2s, "ws2", l * DE, DE, 0, D, r_w2s, s_w[2])
            for j in range(4):
                b1, b3 = j % 2, 2 + j % 2
                for kc in range(KC):
                    mm(psum[:, b1, :], w13[slot][:, 0, kc, j * 128:(j + 1) * 128], hT[:, kc, :], kc == 0, kc == KC - 1,
                       [r_w13[slot], r_hT], [r_ps[b1]])
                for kc in range(KC):
                    mm(psum[:, b3, :], w13[slot][:, 1, kc, j * 128:(j + 1) * 128], hT[:, kc, :], kc == 0, kc == KC - 1,
                       [r_w13[slot], r_hT], [r_ps[b3]])
                act(s1[j % 2], psum[:, b1, :], AF.Silu, [r_ps[b1]], [r_s1[j % 2]])
                vtt(aT[:, j, :], s1[j % 2], psum[:, b3, :], ALU.mult, [r_s1[j % 2], r_ps[b3]], [r_aT])
            yi = 0
            for g in range(4):
                for dh in range(2):
                    yb = 4 + 2 * (yi % 2)
                    yi += 1
                    for ds in range(2):
                        for j in range(4):
                            mm(psum[:, yb + ds, :], aT[:, j, g * 128:(g + 1) * 128], w2s[:, j, (dh * 2 + ds) * 512:(dh * 2 + ds + 1) * 512],
                               j == 0, j == 3, [r_aT, r_w2s], [r_ps[yb + ds]])
                    ysrc = psum[:, yb:yb + 2, :].rearrange("p a f -> p (a f)")
                    adst = acc[:, g, dh * 1024:(dh + 1) * 1024]
                    if e_ == 0:
                        vts(adst, ysrc, gates[:, g, e_:e_ + 1], None, ALU.mult, None, [r_ps[yb], r_ps[yb + 1], r_gates], [r_acc])
                    else:
                        vstt(adst, ysrc, gates[:, g, e_:e_ + 1], adst, ALU.mult, ALU.add, [r_ps[yb], r_ps[yb + 1], r_gates, r_acc], [r_acc])
        for kc in range(KC):
            vts(xT[:, kc, :], xT[:, kc, :], ALPHA, None, ALU.mult, None, [r_xT], [r_xT], eng="gpsimd")
        for dc in range(KC):
            bank = dc % 4
            for g in range(4):
                P.op("tensor", lambda e, bank=bank, g=g, dc=dc: e.transpose(psum[:, bank, g * 128:(g + 1) * 128], acc[:, g, dc * 128:(dc + 1) * 128], ident[:]),
                     reads=[r_acc, r_const], writes=[r_ps[bank]], inc=(g == 3))
            vstt(xT[:, dc, :], psum[:, bank, :], scal[:, gidx, b, dc:dc + 1], xT[:, dc, :], ALU.mult, ALU.add, [r_ps[bank], r_scal, r_xT], [r_xT])

    def gmlp(b):
        for ng in range(8):
            slot = ng % 2
            wload(winb[slot], "win", 0, 2048, ng * 512, 512, r_winb[slot], s_w[3 + slot])
            for nn in range(4):
                n = ng * 4 + nn
                bank = n % 2
                for kc in range(KC):
                    mm(psum[:, bank, :], winb[slot][:, kc, nn * 128:(nn + 1) * 128], hT[:, kc, :], kc == 0, kc == KC - 1,
                       [r_winb[slot], r_hT], [r_ps[bank]])
                act(uT[:, n, :], psum[:, bank, :], AF.Gelu, [r_ps[bank], r_const], [r_uT], bias=binu[:, n:n + 1])
        ws_i = 0
        for g in range(4):
            for vs in range(8):
                slot = ws_i % 2
                ws_i += 1
                wload(winb[slot], "win", 0, 2048, 4096 + vs * 512, 512, r_winb[slot], s_w[3 + slot])
                bsl = binvb[slot]
                srcb = binv_h.ap()[:, vs * 512:(vs + 1) * 512]
                P.dma("gpsimd", lambda e, bsl=bsl, srcb=srcb: e.dma_start(out=bsl[0:1], in_=srcb), s_b[slot], writes=[r_binvb[slot]])
                bank = 2 + vs % 2
                for kc in range(KC):
                    mm(psum[:, bank, :], hT[:, kc, g * 128:(g + 1) * 128], winb[slot][:, kc, :], kc == 0, False,
                       [r_winb[slot], r_hT], [r_ps[bank]])
                mm(psum[:, bank, :], ones16[0:1, :], bsl[0:1], False, True, [r_binvb[slot], r_const], [])
                act(vg32[:, vs * 512:(vs + 1) * 512], psum[:, bank, :], AF.Gelu, [r_ps[bank]], [r_vg32])
                P.op("vector", lambda e, vs=vs: e.bn_stats(out=bnst[:, vs, :], in_=vg32[:, vs * 512:(vs + 1) * 512]), reads=[r_vg32], writes=[r_bnst])
            mv = small[:, 8:10]
            P.op("vector", lambda e: e.bn_aggr(out=mv, in_=bnst.rearrange("p a s -> p (a s)")), reads=[r_bnst], writes=[r_small])
            act(small[:, 10:11], small[:, 9:10], AF.Sqrt, [r_small, r_const], [r_small], bias=epsc[:, 0:1])
            P.op("vector", lambda e: e.reciprocal(out=small[:, 10:11], in_=small[:, 10:11]), reads=[r_small], writes=[r_small])
            vstt(small[:, 11:12], small[:, 8:9], -1.0, small[:, 10:11], ALU.mult, ALU.mult, [r_small], [r_small])
            vl = vln[g % 2]
            rvl = r_vln[g % 2]
            act(vl, vg32, AF.Identity, [r_vg32, r_small], [rvl], bias=small[:, 11:12], scale=small[:, 10:11])
            for fc in range(32):
                g8 = fc // 4
                bank = 4 + fc % 4
                mm(psum[:, bank, 0:128], vl[:, fc * 128:(fc + 1) * 128], wsT[:, g8, :], True, True, [rvl, r_const], [r_ps[bank]])
                gt = gtmp[fc % 4]
                rg = r_gtmp[fc % 4]
                vstt(gt, psum[:, bank, 0:128], vgb[:, fc:fc + 1], Bt[:, fc, :], ALU.mult, ALU.add, [r_ps[bank], r_const], [rg])
                vtt(uT[:, fc, g * 128:(g + 1) * 128], uT[:, fc, g * 128:(g + 1) * 128], gt, ALU.mult, [r_uT, rg], [r_uT], eng="gpsimd")
        for kc in range(KC):
            vts(xT[:, kc, :], xT[:, kc, :], ALPHA, None, ALU.mult, None, [r_xT], [r_xT], eng="gpsimd")
        for dc in range(KC):
            slot = dc % 2
            wload(woutb[slot], "wout", 0, 4096, dc * 128, 128, r_woutb[slot], s_w[slot])
            bank = dc % 2
            for fc in range(32):
                mm(psum[:, bank, :], woutb[slot][:, fc, :], uT[:, fc, :], fc == 0, fc == 31, [r_woutb[slot], r_uT], [r_ps[bank]])
            vstt(xT[:, dc, :], psum[:, bank, :], scal[:, 8, b, dc:dc + 1], xT[:, dc, :], ALU.mult, ALU.add, [r_ps[bank], r_scal, r_xT], [r_xT])

    out_toks = []
    for slot_i, tile in enumerate(tiles):
        b = tile // 4
        load_xT(xT_h, tile * TT)
        modulate(0, 1, b)
        phase_switch()
        attention(tile)
        phase_switch()
        layernorm(0, "mix", b, (3, 4))
        dump(slot_i * 4 + 0)
        phase_switch()
        moe(0, b, slot_i)
        phase_switch()
        layernorm(0, "ffn", b, (6, 7))
        dump(slot_i * 4 + 1)
        phase_switch()
        gmlp(b)
        phase_switch()
        layernorm(1, "mix", b, (9, 10))
        dump(slot_i * 4 + 2)
        phase_switch()
        moe(1, b, slot_i)
        phase_switch()
        layernorm(1, "ffn", b, None)
        dump(slot_i * 4 + 3)
        dst = outT_h.ap()[:, tile * TT:(tile + 1) * TT].rearrange("(k p) t -> p k t", p=128)
        out_toks.append(P.dma("gpsimd", lambda e, dst=dst: e.dma_start(out=dst, in_=xT[:]), s_out, reads=[r_xT]))
    finals = [out_toks[-1]]
    if dbg:
        finals.append(Tok(s_dbg, P.count[s_dbg]))
    finals.append(Tok(s_kvw, P.count[s_kvw]))
    P.finalize(final_waits=finals)
    st.close()
    return nc


def _rope_tables():
    t = np.arange(SEQ)
    row = (t // 64).astype(np.float32)
    col = (t % 64).astype(np.float32)
    inv = (10000.0 ** (-np.arange(0, 32, 2, dtype=np.float32) / 32.0)).astype(np.float32)
    ang_r = row[None, :] * inv[:, None]
    ang_c = col[None, :] * inv[:, None]
    C = np.zeros((64, SEQ), np.float32)
    S_ = np.zeros((64, SEQ), np.float32)
    C[0:16] = np.cos(ang_r); C[16:32] = np.cos(ang_r); C[32:48] = np.cos(ang_c); C[48:64] = np.cos(ang_c)
    S_[0:16] = -np.sin(ang_r); S_[16:32] = np.sin(ang_r); S_[32:48] = -np.sin(ang_c); S_[48:64] = np.sin(ang_c)
    return C, S_


def _swap_cols(w):
    k, n = w.shape
    v = w.reshape(k, n // 64, 4, 16)
    return np.ascontiguousarray(v[:, :, [1, 0, 3, 2], :]).reshape(k, n)


def _fm(v, nch):
    return np.ascontiguousarray(v.reshape(nch, 128).T)


def make_in_maps(x, c, ctx, c_ctx, w_ada, b_ada, ln_mix_g, ln_mix_b, ln_ffn_g, ln_ffn_b,
                 attn_w_qkv, attn_w_o, attn_sink, gmlp_w_in, gmlp_b_in, gmlp_v_g, gmlp_v_b, gmlp_w_s, gmlp_b_s, gmlp_w_o,
                 moe_w_router, moe_bias, moe_w1, moe_w3, moe_w2, moe_ws1, moe_ws3, moe_ws2):
    f = lambda a: np.asarray(a, dtype=np.float32)
    x, c, ctx, c_ctx = f(x), f(c), f(ctx), f(c_ctx)
    wqkv = f(attn_w_qkv)[0]
    wqksw = _swap_cols(wqkv[:, :2304])
    flat = {
        "wada": f(w_ada).reshape(-1), "wqkv": wqkv.reshape(-1), "wqksw": wqksw.reshape(-1), "wo": f(attn_w_o)[0].reshape(-1),
        "win": f(gmlp_w_in)[0].reshape(-1), "wout": f(gmlp_w_o)[0].reshape(-1),
        "ws1": f(moe_ws1).reshape(-1), "ws3": f(moe_ws3).reshape(-1), "ws2": f(moe_ws2).reshape(-1),
        "w1": f(moe_w1).reshape(-1), "w3": f(moe_w3).reshape(-1), "w2": f(moe_w2).reshape(-1),
    }
    ropeC, ropeS = _rope_tables()
    qi = np.arange(128)
    masks = np.zeros((128, 2, 128), np.float32)
    masks[:, 0, :] = (qi[None, :] <= qi[:, None])
    masks[:, 1, :] = (qi[:, None] <= qi[None, :])
    ident = np.eye(128, dtype=np.float32)
    badaT = np.concatenate([_fm(f(b_ada)[l], 96) for l in range(2)], axis=1)
    lnp = np.concatenate([_fm(f(a)[l], 16) for a in (ln_mix_g, ln_mix_b, ln_ffn_g, ln_ffn_b) for l in range(2)], axis=1)
    bin_ = f(gmlp_b_in)[0]
    vgb = np.concatenate([_fm(f(gmlp_v_g)[0], 32), _fm(f(gmlp_v_b)[0], 32)], axis=1)
    wsp = np.ascontiguousarray(f(gmlp_w_s)[0].transpose(1, 0, 2)).reshape(128, 8 * 128)
    common = {
        "badaT": badaT, "lnp": lnp, "wr": f(moe_w_router).reshape(2 * D, NE), "rbias": f(moe_bias), "sink": f(attn_sink),
        "binu": _fm(bin_[:4096], 32), "binv": bin_[4096:].reshape(1, 4096).copy(), "vgb": vgb, "wsp": wsp,
        "bsp": f(gmlp_b_s)[0].reshape(1, 1024).copy(), "ropeC": ropeC, "ropeS": ropeS, "masks": masks.reshape(128, 256), "ident": ident,
    }
    in_maps = []
    for r in range(NCORES):
        m = dict(common)
        m["xT"] = np.ascontiguousarray(x[2 * r:2 * r + 2].reshape(2 * SEQ, D).T)
        m["ctxT"] = np.ascontiguousarray(ctx[2 * r:2 * r + 2].reshape(2 * CTX, D).T)
        m["cT"] = np.ascontiguousarray(np.stack([c[2 * r], c[2 * r + 1], c_ctx], axis=1))
        for g in "ABCE":
            buf = np.empty((GROUP_ROWS[g], 2048), np.float32)
            for name, (gg, off, (R, C)) in WINFO.items():
                if gg != g:
                    continue
                n = R * C // 8
                buf[off:off + n // 2048] = flat[name][r * n:(r + 1) * n].reshape(-1, 2048)
            m["shard" + g] = buf
        in_maps.append(m)
    return in_maps


def kernel(**inputs):
    in_maps = make_in_maps(**inputs)
    nc = build()
    res = run_bass_kernel_spmd(nc, in_maps, core_ids=list(range(NCORES)))
    out = np.empty((16, SEQ, D), np.float32)
    for r in range(NCORES):
        out[2 * r:2 * r + 2] = res.results[r]["outT"].T.reshape(2, SEQ, D)
    return out
```
